# Optimizing a Trainium2 kernel written in Bass

```python
import jax, jax.numpy as jnp
from jax import lax
import numpy as np

D_MODEL = 1024
BATCH = 2
SEQ = 8192
DEPTH = 4

N_MIXERS = 2
N_HGRN_LAYERS = (DEPTH + N_MIXERS - 1) // N_MIXERS
N_CONV_LAYERS = DEPTH // N_MIXERS
HG_DK = 128
HG_HEADS = D_MODEL // HG_DK
HG_DV = D_MODEL // HG_HEADS
HG_CHUNK = 64
HG_STREAMS = 5
CONV_WIDTH = 3
CONV_STREAMS = 3
MOE_GROUPS = 4
MOE_EXPERTS_PER_GROUP = 8
MOE_EXPERTS = MOE_GROUPS * MOE_EXPERTS_PER_GROUP
MOE_TOPK = 2
MOE_D_EXPERT = D_MODEL // 2
MOE_BLOCK = 256
DN_ALPHA = (2.0 * DEPTH) ** 0.25
DN_BETA = (8.0 * DEPTH) ** -0.25
LN_EPS = 1e-5
RMS_EPS = 1e-6

kernel_name = 'hybrid_hgrn2_shortconv_hmoe_deepnorm_encoder'


def _layer_norm(x, g, b):
    xf = x.astype(jnp.float32)
    mu = jnp.mean(xf, axis=-1, keepdims=True)
    var = jnp.mean(jnp.square(xf - mu), axis=-1, keepdims=True)
    return ((xf - mu) * lax.rsqrt(var + LN_EPS) * g + b).astype(x.dtype)


def _gla_chunked(q, k, v, logf):
    b, h, t, dk = q.shape
    dv = v.shape[-1]
    n = t // HG_CHUNK

    def chunks(a):
        return jnp.moveaxis(a.reshape(b, h, n, HG_CHUNK, a.shape[-1]), 2, 0)

    gc = jnp.cumsum(chunks(logf), axis=-2)
    incl = jnp.tril(jnp.ones((HG_CHUNK, HG_CHUNK), dtype=bool))[:, :, None]

    def step(state, inp):
        qc, kc, vc, g = inp
        o_inter = jnp.einsum('bhik,bhkv->bhiv', qc * jnp.exp(g), state)
        diff = g[:, :, :, None, :] - g[:, :, None, :, :]
        decay = jnp.exp(jnp.where(incl, diff, -jnp.inf))
        scores = jnp.einsum('bhik,bhjk,bhijk->bhij', qc, kc, decay)
        o = o_inter + jnp.einsum('bhij,bhjv->bhiv', scores, vc)
        g_last = g[:, :, -1:, :]
        state = (jnp.exp(g_last[:, :, 0, :, None]) * state
                 + jnp.einsum('bhjk,bhjv->bhkv', kc * jnp.exp(g_last - g), vc))
        return state, o

    s0 = jnp.zeros((b, h, dk, dv), jnp.float32)
    _, o = lax.scan(step, s0, (chunks(q), chunks(k), chunks(v), gc))
    return jnp.moveaxis(o, 0, 2).reshape(b, h, t, dv)


def _hgrn2_mixer(x, w_in, lb, norm_w, w_out):
    b, t, d = x.shape
    q, f_fwd, f_bwd, v, gate = jnp.split(x @ w_in, HG_STREAMS, axis=-1)

    def heads(a):
        return a.reshape(b, t, HG_HEADS, -1).transpose(0, 2, 1, 3).astype(jnp.float32)

    def log_forget(fx, lbd):
        lbd = lbd.astype(jnp.float32).reshape(HG_HEADS, 1, HG_DK)
        return jnp.logaddexp(jnp.log(lbd), jnp.log1p(-lbd) + jax.nn.log_sigmoid(heads(fx)))

    q = jax.nn.silu(heads(q))
    v = heads(v)
    lf_f = log_forget(f_fwd, lb[0])
    lf_b = jnp.flip(log_forget(f_bwd, lb[1]), axis=2)
    o_f = _gla_chunked(q, -jnp.expm1(lf_f), v, lf_f)
    o_b = jnp.flip(_gla_chunked(jnp.flip(q, axis=2), -jnp.expm1(lf_b),
                                jnp.flip(v, axis=2), lf_b), axis=2)
    o = (o_f + o_b).transpose(0, 2, 1, 3)
    o = o * lax.rsqrt(jnp.mean(o * o, axis=-1, keepdims=True) + RMS_EPS) * norm_w.astype(jnp.float32)
    o = o.reshape(b, t, d) * jax.nn.silu(gate.astype(jnp.float32))
    return o.astype(x.dtype) @ w_out


def _short_conv_mixer(x, w_in, conv_w, w_out):
    bg, cg, h = jnp.split(x @ w_in, CONV_STREAMS, axis=-1)
    half = (CONV_WIDTH - 1) // 2
    y = lax.conv_general_dilated(cg * h, conv_w[:, None, :], window_strides=(1,),
                                 padding=[(half, half)],
                                 dimension_numbers=('NWC', 'WIO', 'NWC'),
                                 feature_group_count=x.shape[-1])
    return (bg * y) @ w_out


def _hier_moe(x, w_group, b_group, w_expert, b_expert, w_up, w_down):
    b, t, d = x.shape
    n = b * t
    xf = x.reshape(n, d)
    xr = xf.astype(jnp.float32)
    g_prob = jax.nn.softmax(xr @ w_group.astype(jnp.float32) + b_group.astype(jnp.float32), axis=-1)
    p_group, g_sel = lax.top_k(g_prob, 1)
    e_logits = (xr @ w_expert.astype(jnp.float32) + b_expert.astype(jnp.float32)
                ).reshape(n, MOE_GROUPS, MOE_EXPERTS_PER_GROUP)
    e_logits = jnp.take_along_axis(e_logits, g_sel[:, :, None], axis=1)[:, 0]
    top_logit, top_local = lax.top_k(e_logits, MOE_TOPK)
    gates = p_group * jax.nn.softmax(top_logit, axis=-1)
    expert = g_sel * MOE_EXPERTS_PER_GROUP + top_local

    nk = n * MOE_TOPK
    e_flat = expert.reshape(nk).astype(jnp.int32)
    tok_flat = jnp.arange(nk, dtype=jnp.int32) // MOE_TOPK
    order = jnp.argsort(e_flat)
    e_sorted = e_flat[order]
    counts = jnp.zeros((MOE_EXPERTS,), jnp.int32).at[e_flat].add(1)
    padded = (counts + MOE_BLOCK - 1) // MOE_BLOCK * MOE_BLOCK
    start = jnp.cumsum(counts) - counts
    pad_end = jnp.cumsum(padded)
    pad_start = pad_end - padded
    dest = pad_start[e_sorted] + jnp.arange(nk, dtype=jnp.int32) - start[e_sorted]
    n_rows = -(-nk // MOE_BLOCK) * MOE_BLOCK + MOE_EXPERTS * MOE_BLOCK
    n_blocks = n_rows // MOE_BLOCK
    row_tok = jnp.full((n_rows,), n, jnp.int32).at[dest].set(tok_flat[order])
    row_gate = jnp.zeros((n_rows,), jnp.float32).at[dest].set(gates.reshape(nk)[order])
    block_start = jnp.arange(n_blocks, dtype=jnp.int32) * MOE_BLOCK
    block_expert = jnp.minimum(jnp.searchsorted(pad_end, block_start, side='right'),
                               MOE_EXPERTS - 1).astype(jnp.int32)
    x_pad = jnp.concatenate([xf, jnp.zeros((1, d), xf.dtype)], axis=0)
    xs = x_pad[row_tok].reshape(n_blocks, MOE_BLOCK, d)

    def run_block(args):
        xb, e = args
        hg, hu = jnp.split(xb @ w_up[e], 2, axis=-1)
        return (jax.nn.silu(hg) * hu) @ w_down[e]

    ys = lax.map(run_block, (xs, block_expert)).reshape(n_rows, d)
    out = jnp.zeros((n + 1, d), x.dtype).at[row_tok].add(ys * row_gate[:, None].astype(x.dtype))
    return out[:n].reshape(b, t, d)


def setup_inputs(seed: int = 0) -> dict:
    key = jax.random.key(seed)
    ks = jax.random.split(key, 18)
    d = D_MODEL

    def nrm(k, shape, scale):
        return jax.random.normal(k, shape, jnp.float32) * scale

    return {
        'x': nrm(ks[0], (BATCH, SEQ, d), 1.0),
        'hg_w_in': nrm(ks[1], (N_HGRN_LAYERS, d, HG_STREAMS * d), d ** -0.5),
        'hg_lb_logits': nrm(ks[2], (DEPTH, 2, HG_HEADS * HG_DK), 0.5),
        'hg_norm_w': 1.0 + nrm(ks[3], (N_HGRN_LAYERS, HG_DV), 0.02),
        'hg_w_out': nrm(ks[4], (N_HGRN_LAYERS, d, d), DN_BETA * d ** -0.5),
        'cv_w_in': nrm(ks[5], (N_CONV_LAYERS, d, CONV_STREAMS * d), d ** -0.5),
        'cv_w': nrm(ks[6], (N_CONV_LAYERS, CONV_WIDTH, d), CONV_WIDTH ** -0.5),
        'cv_w_out': nrm(ks[7], (N_CONV_LAYERS, d, d), DN_BETA * d ** -0.5),
        'ln_g': 1.0 + nrm(ks[8], (DEPTH, 2, d), 0.02),
        'ln_b': nrm(ks[9], (DEPTH, 2, d), 0.02),
        'moe_w_group': nrm(ks[10], (DEPTH, d, MOE_GROUPS), d ** -0.5),
        'moe_b_group': nrm(ks[11], (DEPTH, MOE_GROUPS), 0.01),
        'moe_w_expert': nrm(ks[12], (DEPTH, d, MOE_EXPERTS), d ** -0.5),
        'moe_b_expert': nrm(ks[13], (DEPTH, MOE_EXPERTS), 0.01),
        'moe_w_up': nrm(ks[14], (DEPTH, MOE_EXPERTS, d, 2 * MOE_D_EXPERT), d ** -0.5),
        'moe_w_down': nrm(ks[15], (DEPTH, MOE_EXPERTS, MOE_D_EXPERT, d), DN_BETA * MOE_D_EXPERT ** -0.5),
    }


def reference(x, hg_w_in, hg_lb_logits, hg_norm_w, hg_w_out, cv_w_in, cv_w, cv_w_out,
              ln_g, ln_b, moe_w_group, moe_b_group, moe_w_expert, moe_b_expert,
              moe_w_up, moe_w_down):
    lb_all = jnp.cumsum(jax.nn.softmax(hg_lb_logits.astype(jnp.float32), axis=0), axis=0)
    lb_all = lb_all - lb_all[0:1]
    for layer in range(DEPTH):
        j = layer // N_MIXERS
        if layer % N_MIXERS == 0:
            mix = _hgrn2_mixer(x, hg_w_in[j], lb_all[layer], hg_norm_w[j], hg_w_out[j])
        else:
            mix = _short_conv_mixer(x, cv_w_in[j], cv_w[j], cv_w_out[j])
        x = _layer_norm(DN_ALPHA * x + mix, ln_g[layer, 0], ln_b[layer, 0])
        ffn = _hier_moe(x, moe_w_group[layer], moe_b_group[layer], moe_w_expert[layer],
                        moe_b_expert[layer], moe_w_up[layer], moe_w_down[layer])
        x = _layer_norm(DN_ALPHA * x + ffn, ln_g[layer, 1], ln_b[layer, 1])
    return x
```

```python
from contextlib import ExitStack
import numpy as np
import concourse.bass as bass
import concourse.mybir as mybir
from concourse.bass_utils import run_bass_kernel_spmd

F32 = mybir.dt.float32
BF16 = mybir.dt.bfloat16
AF = mybir.ActivationFunctionType
ALU = mybir.AluOpType
AX = mybir.AxisListType

NCORES = 8
TOK = 2048
NT = 16
D = 1024
KC = 8
DEPTH = 4
NE = 32
DE = 512
ALPHA = float((2.0 * DEPTH) ** 0.25)
LN_EPS = 1e-5
RMS_EPS = 1e-6
CH = 64
NCH = TOK // CH

ENGS = ("tensor", "vector", "scalar", "gpsimd", "sync")
N_DMA_SEMS = 12
DMA_QUEUES = ("sync", "gpsimd")


class Dep:
    __slots__ = ("writers", "readers")

    def __init__(self):
        self.writers = {}
        self.readers = []


def deps(n):
    return [Dep() for _ in range(n)]


class Prog:
    def __init__(self, nc):
        self.nc = nc
        self.epoch = 0
        self._reset()

    def _reset(self):
        nc = self.nc
        self.ops = {e: [] for e in ENGS}
        self.cnt = {e: 0 for e in ENGS}
        self.semh = {}
        for e in ENGS:
            self.semh[("eng", e)] = nc.alloc_semaphore("s_%s_%d" % (e, self.epoch))
        self.dcnt = {}
        self.dnext = {}
        for q in DMA_QUEUES:
            for i in range(N_DMA_SEMS):
                self.semh[("dma", q, i)] = nc.alloc_semaphore("d%s%d_%d" % (q[0], i, self.epoch))
                self.dcnt[(q, i)] = 0
            self.dnext[q] = 0
        self.semh[("cc", 0)] = nc.alloc_semaphore("cc_%d" % self.epoch)
        self.ccnt = 0
        self.seen = {e: {} for e in ENGS}

    def _collect(self, eng, rd, wr):
        evs = []
        ep = self.epoch
        for d in rd:
            for lw in d.writers.values():
                if lw[3] == ep:
                    evs.append(lw)
        for d in wr:
            for lw in d.writers.values():
                if lw[3] == ep and (lw[0][0] != "eng" or lw[2] != eng):
                    evs.append(lw)
            for r in d.readers:
                if r[3] == ep and (r[0][0] != "eng" or r[2] != eng):
                    evs.append(r)
        waits = {}
        seen = self.seen[eng]
        for key, val, _, _ in evs:
            if seen.get(key, 0) >= val:
                continue
            if key not in waits or waits[key] < val:
                waits[key] = val
        for key, val in waits.items():
            seen[key] = val
        return [(self.semh[key], val) for key, val in waits.items()]

    def _record(self, ev, rd, wr):
        for d in wr:
            d.writers[ev[0]] = ev
            d.readers = []
        for d in rd:
            d.readers.append(ev)

    def op(self, eng, fn, rd=(), wr=(), inc=True):
        waits = self._collect(eng, rd, wr)
        if not inc:
            def run0(e, fn=fn, waits=waits):
                for s, v in waits:
                    e.wait_ge(s, v)
                fn(e)
            self.ops[eng].append(run0)
            return
        self.cnt[eng] += 1
        key = ("eng", eng)
        ev = (key, self.cnt[eng], eng, self.epoch)
        sem = self.semh[key]

        def run(e, fn=fn, waits=waits, sem=sem):
            for s, v in waits:
                e.wait_ge(s, v)
            fn(e).then_inc(sem, 1)
        self.ops[eng].append(run)
        self._record(ev, rd, wr)

    def dma(self, q, fn, rd=(), wr=()):
        waits = self._collect(q, rd, wr)
        j = self.dnext[q]
        self.dnext[q] = (j + 1) % N_DMA_SEMS
        key = ("dma", q, j)
        prev = self.dcnt[(q, j)]
        if prev > 0 and self.seen[q].get(key, 0) < prev:
            waits.append((self.semh[key], prev))
            self.seen[q][key] = prev
        self.dcnt[(q, j)] = prev + 16
        ev = (key, self.dcnt[(q, j)], q, self.epoch)
        sem = self.semh[key]

        def run(e, fn=fn, waits=waits, sem=sem):
            for s, v in waits:
                e.wait_ge(s, v)
            fn(e).then_inc(sem, 16)
        self.ops[q].append(run)
        self._record(ev, rd, wr)

    def coll(self, fn, rd=(), wr=()):
        q = "gpsimd"
        waits = self._collect(q, rd, wr)
        key = ("cc", 0)
        self.ccnt += 1
        val = self.ccnt
        ev = (key, val, q, self.epoch)
        sem = self.semh[key]

        def run(e, fn=fn, waits=waits, sem=sem, val=val):
            for s, v in waits:
                e.wait_ge(s, v)
            fn(e).then_inc(sem)
            e.wait_ge(sem, val)
        self.seen[q][key] = val
        self.ops[q].append(run)
        self._record(ev, rd, wr)

    def flush(self):
        targets = {}
        for e in ENGS:
            if self.cnt[e] > 0:
                targets[("eng", e)] = self.cnt[e]
        for (q, j), v in self.dcnt.items():
            if v > 0:
                targets[("dma", q, j)] = v
        if self.ccnt > 0:
            targets[("cc", 0)] = self.ccnt
        for e in ENGS:
            waits = []
            for key, val in targets.items():
                if key == ("eng", e):
                    continue
                if self.seen[e].get(key, 0) >= val:
                    continue
                self.seen[e][key] = val
                waits.append((self.semh[key], val))

            def run(eo, waits=waits):
                for s_, v in waits:
                    eo.wait_ge(s_, v)
            if waits:
                self.ops[e].append(run)

    def finish(self):
        nc = self.nc
        self.flush()
        with nc.Block() as block:
            for name in ENGS:
                lst = self.ops[name]

                def body(e, lst=lst):
                    for r in lst:
                        r(e)
                getattr(block, name)(body)

    def mm(self, out, lhsT, rhs, start, stop, rd, wr, inc=None):
        self.op("tensor", lambda e: e.matmul(out, lhsT, rhs, start=start, stop=stop), rd, wr,
                inc=bool(stop) if inc is None else inc)

    def tp(self, out, in_, ident, rd, wr):
        self.op("tensor", lambda e: e.transpose(out, in_, ident), rd, wr)

    def act(self, out, in_, func, rd, wr, bias=None, scale=None):
        kw = {}
        if bias is not None:
            kw["bias"] = bias
        if scale is not None:
            kw["scale"] = scale
        self.op("scalar", lambda e: e.activation(out, in_, func, **kw), rd, wr)

    def ts(self, eng, out, in0, s1, s2, op0, op1, rd, wr):
        if op1 is None:
            self.op(eng, lambda e: e.tensor_scalar(out, in0, s1, None, op0), rd, wr)
        else:
            self.op(eng, lambda e: e.tensor_scalar(out, in0, s1, s2, op0, op1), rd, wr)

    def tt(self, eng, out, in0, in1, op, rd, wr):
        self.op(eng, lambda e: e.tensor_tensor(out, in0, in1, op), rd, wr)

    def stt(self, eng, out, in0, scalar, in1, op0, op1, rd, wr):
        self.op(eng, lambda e: e.scalar_tensor_tensor(out, in0, scalar, in1, op0, op1), rd, wr)

    def cp(self, eng, out, in_, rd, wr):
        if eng == "scalar":
            self.op(eng, lambda e: e.copy(out, in_), rd, wr)
        else:
            self.op(eng, lambda e: e.tensor_copy(out, in_), rd, wr)

    def red(self, eng, out, in_, op, rd, wr):
        self.op(eng, lambda e: e.tensor_reduce(out, in_, AX.X, op), rd, wr)

    def ld(self, q, out, in_, rd, wr):
        self.dma(q, lambda e: e.dma_start(out=out, in_=in_), rd, wr)


class Ctx:
    pass


_UID = [0]


def sbt(st, nc, name, shape, dt):
    _UID[0] += 1
    return st.enter_context(nc.sbuf_tensor("%s_%d" % (name, _UID[0]), list(shape), dt))


def emit_layernorm_store(P, C, st_tiles, y, d_y, gam, bet, d_gb, xout, d_xo_t, t):
    stats, d_st, mv, d_mv = st_tiles
    P.op("vector", lambda e: e.bn_stats(stats[:, 0, :], y[:, 0:512]), [d_y], [d_st])
    P.op("vector", lambda e: e.bn_stats(stats[:, 1, :], y[:, 512:1024]), [d_y], [d_st])
    P.op("vector", lambda e: e.bn_aggr(mv[:, 0:2], stats[:, :, :].rearrange("p a b -> p (a b)")), [d_st], [d_mv])
    P.ts("vector", mv[:, 2:3], mv[:, 1:2], LN_EPS, None, ALU.add, None, [d_mv], [d_mv])
    P.act(mv[:, 2:3], mv[:, 2:3], AF.Sqrt, [d_mv], [d_mv])
    P.op("vector", lambda e: e.reciprocal(mv[:, 3:4], mv[:, 2:3]), [d_mv], [d_mv])
    P.stt("vector", y, y, mv[:, 0:1], gam, ALU.subtract, ALU.mult, [d_y, d_mv, d_gb], [d_y])
    P.stt("vector", y, y, mv[:, 3:4], bet, ALU.mult, ALU.add, [d_y, d_mv, d_gb], [d_y])
    P.ld("sync", xout[t * 128:(t + 1) * 128, :], y, [d_y], [d_xo_t])


def emit_load_gb(P, C, st, L, which):
    nc = P.nc
    gam = sbt(st, nc, "gam", [128, D], F32)
    bet = sbt(st, nc, "bet", [128, D], F32)
    d_gb = Dep()
    P.ld("sync", gam[:], C.ln_g[L, which, :].partition_broadcast(128), [], [d_gb])
    P.ld("sync", bet[:], C.ln_b[L, which, :].partition_broadcast(128), [], [d_gb])
    return gam, bet, d_gb


def emit_mixer_tail(P, C, st, L, actT, d_act, w_out_ap, xin, d_xi, xout, d_xo):
    nc = P.nc
    wo = sbt(st, nc, "wo", [128, KC, D], BF16)
    d_wo = Dep()
    P.ld("gpsimd", wo[:], w_out_ap.rearrange("(k p) c -> p k c", p=128), [], [d_wo])
    gam, bet, d_gb = emit_load_gb(P, C, st, L, 0)
    xt = [sbt(st, nc, "txt%d" % i, [128, D], F32) for i in range(2)]
    d_xt = deps(2)
    stats = [sbt(st, nc, "tst%d" % i, [128, 2, 6], F32) for i in range(2)]
    mv = [sbt(st, nc, "tmv%d" % i, [128, 4], F32) for i in range(2)]
    d_st = deps(2)
    d_mv = deps(2)
    for t in range(NT):
        b = t % 2
        P.ld("sync", xt[b][:], xin[t * 128:(t + 1) * 128, :], [d_xi[t]], [d_xt[b]])
        for n in range(2):
            ps = C.ps[n]
            for k in range(KC):
                P.mm(ps[:, :], actT[:, k, t * 128:(t + 1) * 128], wo[:, k, n * 512:(n + 1) * 512],
                     k == 0, k == KC - 1, [d_act, d_wo], [C.dps[n]])
            P.stt("vector", xt[b][:, n * 512:(n + 1) * 512], xt[b][:, n * 512:(n + 1) * 512], ALPHA, ps[:, :],
                  ALU.mult, ALU.add, [d_xt[b], C.dps[n]], [d_xt[b]])
        emit_layernorm_store(P, C, (stats[b], d_st[b], mv[b], d_mv[b]), xt[b][:], d_xt[b], gam[:], bet[:], d_gb,
                             xout, d_xo[t], t)


def emit_load_xT(P, C, st, xin, d_xi, xT, d_xT, col0=0, per_tile=None):
    nc = P.nc
    xt = [sbt(st, nc, "lxt%d" % i, [128, D], F32) for i in range(2)]
    d_xt = deps(2)
    for t in range(NT):
        b = t % 2
        P.ld("sync", xt[b][:], xin[t * 128:(t + 1) * 128, :], [d_xi[t]], [d_xt[b]])
        for h in range(2):
            ps = C.ps[2 + h]
            dp = C.dps[2 + h]
            for kk in range(4):
                k = h * 4 + kk
                P.tp(ps[:, kk * 128:(kk + 1) * 128], xt[b][:, k * 128:(k + 1) * 128], C.ident[:], [d_xt[b], C.d_const],
                     [dp])
            dst = xT[:, h * 4:(h + 1) * 4, col0 + t * 128:col0 + (t + 1) * 128]
            src = ps[:, :].rearrange("p (a b) -> p a b", a=4)
            if h == 0:
                P.cp("scalar", dst, src, [dp], [d_xT])
            else:
                P.cp("vector", dst, src, [dp], [d_xT])
            if per_tile is not None:
                per_tile(t, b, h, xt[b], d_xt[b], ps, dp)


def emit_moe(P, C, L, xin, d_xi, xout, d_xo):
    nc = P.nc
    with ExitStack() as st:
        acc = sbt(st, nc, "acc", [128, NT, D], F32)
        d_acc = deps(NT)
        xT = sbt(st, nc, "xT", [128, KC, TOK], BF16)
        d_xT = Dep()
        gates = sbt(st, nc, "gates", [128, NT, NE], F32)
        d_gates = Dep()
        wr = sbt(st, nc, "wr", [128, KC, 36], F32)
        rb = sbt(st, nc, "rb", [128, 36], F32)
        d_wr = Dep()
        P.ld("sync", wr[:, :, 0:4], C.moe_w_group[C.li(L)].rearrange("(k p) g -> p k g", p=128), [], [d_wr])
        P.ld("sync", wr[:, :, 4:36], C.moe_w_expert[C.li(L)].rearrange("(k p) g -> p k g", p=128), [], [d_wr])
        P.ld("sync", rb[:, 0:4], C.moe_b_group[C.li(L), :].partition_broadcast(128), [], [d_wr])
        P.ld("sync", rb[:, 4:36], C.moe_b_expert[C.li(L), :].partition_broadcast(128), [], [d_wr])
        xT32 = [sbt(st, nc, "xT32_%d" % i, [128, KC, 128], F32) for i in range(2)]
        d_x32 = deps(2)
        lg = sbt(st, nc, "lg", [128, NT, 36], F32)
        d_lg = Dep()

        def per_tile(t, b, h, xt, d_xt, ps, dp):
            src = ps[:, :].rearrange("p (a b) -> p a b", a=4)
            eng = "vector" if h == 0 else "scalar"
            P.cp(eng, xT32[b][:, h * 4:(h + 1) * 4, :], src, [dp], [d_x32[b]])
            if h == 1:
                P.ts("gpsimd", acc[:, t, :], xt[:], ALPHA, None, ALU.mult, None, [d_xt], [d_acc[t]])
                psr = C.ps[4]
                for k in range(KC):
                    P.mm(psr[:, 0:36], xT32[b][:, k, :], wr[:, k, :], k == 0, k == KC - 1, [d_x32[b], d_wr],
                         [C.dps[4]])
                P.tt("vector", lg[:, t, :], psr[:, 0:36], rb[:], ALU.add, [C.dps[4], d_wr], [d_lg])

        emit_load_xT(P, C, st, xin, d_xi, xT, d_xT, 0, per_tile)

        def s(name, shape):
            return sbt(st, nc, name, shape, F32)
        gmax = s("gmax", [128, NT]); gsh = s("gsh", [128, NT, 4]); ge = s("ge", [128, NT, 4])
        gs = s("gs", [128, NT]); pg = s("pg", [128, NT]); goh = s("goh", [128, NT, 4])
        em = s("em", [128, NT, NE]); t1 = s("t1", [128, NT, NE]); top1 = s("top1", [128, NT])
        em2 = s("em2", [128, NT, NE]); top2 = s("top2", [128, NT]); sel = s("sel", [128, NT, NE])
        ex = s("ex", [128, NT, NE]); den = s("den", [128, NT])
        dr = Dep()
        V = "vector"
        lgg = lg[:, :, 0:4]
        lge = lg[:, :, 4:36]

        def bc(ap2, n):
            return ap2.unsqueeze(2).to_broadcast([128, NT, n])
        P.red(V, gmax[:], lgg, ALU.max, [d_lg], [dr])
        P.tt(V, gsh[:], lgg, bc(gmax[:], 4), ALU.subtract, [d_lg, dr], [dr])
        P.act(ge[:], gsh[:], AF.Exp, [dr], [dr])
        P.red(V, gs[:], ge[:], ALU.add, [dr], [dr])
        P.op(V, lambda e: e.reciprocal(pg[:], gs[:]), [dr], [dr])
        P.ts(V, goh[:], gsh[:], 0.0, None, ALU.is_ge, None, [dr], [dr])
        emv = em[:].rearrange("p t (g e) -> p t g e", g=4)
        t1v = t1[:].rearrange("p t (g e) -> p t g e", g=4)
        gohb = goh[:].unsqueeze(3).to_broadcast([128, NT, 4, 8])
        lgev = lge.rearrange("p t (g e) -> p t g e", g=4)
        P.tt(V, emv, lgev, gohb, ALU.mult, [d_lg, dr], [dr])
        P.ts(V, t1v, gohb, 1e30, -1e30, ALU.mult, ALU.add, [dr], [dr])
        P.tt(V, em[:], em[:], t1[:], ALU.add, [dr], [dr])
        P.red(V, top1[:], em[:], ALU.max, [dr], [dr])
        P.tt(V, t1[:], em[:], bc(top1[:], NE), ALU.is_ge, [dr], [dr])
        P.ts(V, t1[:], t1[:], -1e30, None, ALU.mult, None, [dr], [dr])
        P.tt(V, em2[:], em[:], t1[:], ALU.add, [dr], [dr])
        P.red(V, top2[:], em2[:], ALU.max, [dr], [dr])
        P.tt(V, sel[:], em[:], bc(top2[:], NE), ALU.is_ge, [dr], [dr])
        P.tt(V, ex[:], em[:], bc(top1[:], NE), ALU.subtract, [dr], [dr])
        P.ts(V, ex[:], ex[:], -80.0, None, ALU.max, None, [dr], [dr])
        P.act(ex[:], ex[:], AF.Exp, [dr], [dr])
        P.tt(V, ex[:], ex[:], sel[:], ALU.mult, [dr], [dr])
        P.red(V, den[:], ex[:], ALU.add, [dr], [dr])
        P.op(V, lambda e: e.reciprocal(den[:], den[:]), [dr], [dr])
        P.tt(V, den[:], den[:], pg[:], ALU.mult, [dr], [dr])
        P.tt(V, gates[:], ex[:], bc(den[:], NE), ALU.mult, [dr], [d_gates])

        wup = [sbt(st, nc, "wup%d" % i, [128, KC, 2 * DE], BF16) for i in range(2)]
        wdn = [sbt(st, nc, "wdn%d" % i, [128, 4, D], BF16) for i in range(2)]
        d_wup = deps(2)
        d_wdn = deps(2)
        hT = [sbt(st, nc, "hT%d" % i, [128, 4, 512], BF16) for i in range(2)]
        d_hT = deps(2)
        sg = [sbt(st, nc, "sg%d" % i, [128, 512], F32) for i in range(2)]
        d_sg = deps(2)
        for e_ in range(NE):
            wb = e_ % 2
            P.ld("gpsimd", wup[wb][:], C.moe_w_up[C.li(L), e_].rearrange("(k p) c -> p k c", p=128), [], [d_wup[wb]])
            P.ld("gpsimd", wdn[wb][:], C.moe_w_down[C.li(L), e_].rearrange("(k p) c -> p k c", p=128), [], [d_wdn[wb]])
            for g in range(4):
                hb = (e_ * 4 + g) % 2
                for m in range(4):
                    i = m % 2
                    psg = C.ps[i]
                    psu = C.ps[2 + i]
                    for k in range(KC):
                        P.mm(psg[:, :], wup[wb][:, k, m * 128:(m + 1) * 128], xT[:, k, g * 512:(g + 1) * 512],
                             k == 0, k == KC - 1, [d_wup[wb], d_xT], [C.dps[i]])
                    for k in range(KC):
                        P.mm(psu[:, :], wup[wb][:, k, DE + m * 128:DE + (m + 1) * 128],
                             xT[:, k, g * 512:(g + 1) * 512], k == 0, k == KC - 1, [d_wup[wb], d_xT], [C.dps[2 + i]])
                    P.act(sg[i][:], psg[:, :], AF.Silu, [C.dps[i]], [d_sg[i]])
                    P.tt("vector", hT[hb][:, m, :], sg[i][:], psu[:, :], ALU.mult, [d_sg[i], C.dps[2 + i]],
                         [d_hT[hb]])
                for tt_ in range(4):
                    t = g * 4 + tt_
                    for n in range(2):
                        j = 4 + (tt_ * 2 + n) % 4
                        psy = C.ps[j]
                        for m in range(4):
                            P.mm(psy[:, :], hT[hb][:, m, tt_ * 128:(tt_ + 1) * 128],
                                 wdn[wb][:, m, n * 512:(n + 1) * 512], m == 0, m == 3, [d_hT[hb], d_wdn[wb]],
                                 [C.dps[j]])
                        P.stt("vector", acc[:, t, n * 512:(n + 1) * 512], psy[:, :], gates[:, t, e_:e_ + 1],
                              acc[:, t, n * 512:(n + 1) * 512], ALU.mult, ALU.add, [C.dps[j], d_gates, d_acc[t]],
                              [d_acc[t]])

        gam, bet, d_gb = emit_load_gb(P, C, st, L, 1)
        stats = [sbt(st, nc, "mst%d" % i, [128, 2, 6], F32) for i in range(2)]
        mv = [sbt(st, nc, "mmv%d" % i, [128, 4], F32) for i in range(2)]
        d_st = deps(2)
        d_mv = deps(2)
        for t in range(NT):
            b = t % 2
            emit_layernorm_store(P, C, (stats[b], d_st[b], mv[b], d_mv[b]), acc[:, t, :], d_acc[t], gam[:], bet[:],
                                 d_gb, xout, d_xo[t], t)
        P.flush()


CAP = 256


def emit_moe_sparse(P, C, L, xin, d_xi, xout, d_xo):
    nc = P.nc
    V = "vector"
    with ExitStack() as st:
        acc = sbt(st, nc, "acc", [128, NT, D], F32)
        d_acc = deps(NT)
        gates = sbt(st, nc, "gates", [128, NT, NE], F32)
        sel = sbt(st, nc, "sel", [128, NT, NE], F32)
        rank = sbt(st, nc, "rank", [128, NT, NE], F32)
        idxg = sbt(st, nc, "idxg", [128, NE, 2, 2], F32)
        idxi = sbt(st, nc, "idxi", [128, NE, 2], mybir.dt.int32)
        slot = sbt(st, nc, "slot", [128, NT, 2], mybir.dt.int32)
        d_gates, d_rank, d_idx, d_slot = Dep(), Dep(), Dep(), Dep()
        with ExitStack() as st1:
            wr = sbt(st1, nc, "wr", [128, KC, 36], F32)
            rb = sbt(st1, nc, "rb", [128, 36], F32)
            d_wr = Dep()
            P.ld("sync", wr[:, :, 0:4], C.moe_w_group[C.li(L)].rearrange("(k p) g -> p k g", p=128), [], [d_wr])
            P.ld("sync", wr[:, :, 4:36], C.moe_w_expert[C.li(L)].rearrange("(k p) g -> p k g", p=128), [], [d_wr])
            P.ld("sync", rb[:, 0:4], C.moe_b_group[C.li(L), :].partition_broadcast(128), [], [d_wr])
            P.ld("sync", rb[:, 4:36], C.moe_b_expert[C.li(L), :].partition_broadcast(128), [], [d_wr])
            xT32 = [sbt(st1, nc, "xT32_%d" % i, [128, KC, 128], F32) for i in range(2)]
            d_x32 = deps(2)
            lg = sbt(st1, nc, "lg", [128, NT, 36], F32)
            d_lg = Dep()
            xt = [sbt(st1, nc, "mxt%d" % i, [128, D], F32) for i in range(2)]
            d_xt = deps(2)
            for t in range(NT):
                b = t % 2
                P.ld("sync", xt[b][:], xin[t * 128:(t + 1) * 128, :], [d_xi[t]], [d_xt[b]])
                for h in range(2):
                    ps = C.ps[2 + h]
                    dp = C.dps[2 + h]
                    for kk in range(4):
                        k = h * 4 + kk
                        P.tp(ps[:, kk * 128:(kk + 1) * 128], xt[b][:, k * 128:(k + 1) * 128], C.ident[:],
                             [d_xt[b], C.d_const], [dp])
                    src = ps[:, :].rearrange("p (a b) -> p a b", a=4)
                    P.cp("vector" if h == 0 else "scalar", xT32[b][:, h * 4:(h + 1) * 4, :], src, [dp], [d_x32[b]])
                P.act(acc[:, t, :], xt[b][:], AF.Copy, [d_xt[b]], [d_acc[t]], scale=ALPHA)
                psr = C.ps[4]
                for k in range(KC):
                    P.mm(psr[:, 0:36], xT32[b][:, k, :], wr[:, k, :], k == 0, k == KC - 1, [d_x32[b], d_wr],
                         [C.dps[4]])
                P.tt(V, lg[:, t, :], psr[:, 0:36], rb[:], ALU.add, [C.dps[4], d_wr], [d_lg])

            def s_(name, shape):
                return sbt(st1, nc, name, shape, F32)
            gmax = s_("gmax", [128, NT]); gsh = s_("gsh", [128, NT, 4]); ge = s_("ge", [128, NT, 4])
            gs = s_("gs", [128, NT]); pg = s_("pg", [128, NT]); goh = s_("goh", [128, NT, 4])
            em = s_("em", [128, NT, NE]); t1 = s_("t1", [128, NT, NE]); top1 = s_("top1", [128, NT])
            em2 = s_("em2", [128, NT, NE]); top2 = s_("top2", [128, NT])
            ex = s_("ex", [128, NT, NE]); den = s_("den", [128, NT])
            dr = Dep()
            lgg = lg[:, :, 0:4]
            lge = lg[:, :, 4:36]

            def bc(ap2, n):
                return ap2.unsqueeze(2).to_broadcast([128, NT, n])
            P.red(V, gmax[:], lgg, ALU.max, [d_lg], [dr])
            P.tt(V, gsh[:], lgg, bc(gmax[:], 4), ALU.subtract, [d_lg, dr], [dr])
            P.act(ge[:], gsh[:], AF.Exp, [dr], [dr])
            P.red(V, gs[:], ge[:], ALU.add, [dr], [dr])
            P.op(V, lambda e: e.reciprocal(pg[:], gs[:]), [dr], [dr])
            P.ts(V, goh[:], gsh[:], 0.0, None, ALU.is_ge, None, [dr], [dr])
            emv = em[:].rearrange("p t (g e) -> p t g e", g=4)
            t1v = t1[:].rearrange("p t (g e) -> p t g e", g=4)
            gohb = goh[:].unsqueeze(3).to_broadcast([128, NT, 4, 8])
            lgev = lge.rearrange("p t (g e) -> p t g e", g=4)
            P.tt(V, emv, lgev, gohb, ALU.mult, [d_lg, dr], [dr])
            P.ts(V, t1v, gohb, 1e30, -1e30, ALU.mult, ALU.add, [dr], [dr])
            P.tt(V, em[:], em[:], t1[:], ALU.add, [dr], [dr])
            P.red(V, top1[:], em[:], ALU.max, [dr], [dr])
            P.tt(V, t1[:], em[:], bc(top1[:], NE), ALU.is_ge, [dr], [dr])
            P.ts(V, t1[:], t1[:], -1e30, None, ALU.mult, None, [dr], [dr])
            P.tt(V, em2[:], em[:], t1[:], ALU.add, [dr], [dr])
            P.red(V, top2[:], em2[:], ALU.max, [dr], [dr])
            P.tt(V, sel[:], em[:], bc(top2[:], NE), ALU.is_ge, [dr], [d_gates])
            P.tt(V, ex[:], em[:], bc(top1[:], NE), ALU.subtract, [dr], [dr])
            P.ts(V, ex[:], ex[:], -80.0, None, ALU.max, None, [dr], [dr])
            P.act(ex[:], ex[:], AF.Exp, [dr], [dr])
            P.tt(V, ex[:], ex[:], sel[:], ALU.mult, [dr, d_gates], [dr])
            P.red(V, den[:], ex[:], ALU.add, [dr], [dr])
            P.op(V, lambda e: e.reciprocal(den[:], den[:]), [dr], [dr])
            P.tt(V, den[:], den[:], pg[:], ALU.mult, [dr], [dr])
            P.tt(V, gates[:], ex[:], bc(den[:], NE), ALU.mult, [dr], [d_gates])

            selb = sbt(st1, nc, "selb", [128, NT, NE], BF16)
            onesb = sbt(st1, nc, "onesb", [128, 128], BF16)
            ustb = sbt(st1, nc, "ustb", [128, 128], BF16)
            cnt = s_("cnt", [128, NT, NE])
            base = s_("base", [128, NT, NE])
            P.cp(V, selb[:], sel[:], [d_gates], [dr])
            P.op("gpsimd", lambda e: e.memset(onesb[:], 1.0), [], [dr])
            P.cp("gpsimd", ustb[:], C.ustrict[:], [C.d_const], [dr])
            for t in range(NT):
                P.mm(C.ps[0][:, t * NE:(t + 1) * NE], onesb[:], selb[:, t, :], True, True, [dr], [C.dps[0]])
                P.mm(C.ps[1][:, t * NE:(t + 1) * NE], ustb[:], selb[:, t, :], True, True, [dr], [C.dps[1]])
            P.cp(V, cnt[:].rearrange("p t e -> p (t e)"), C.ps[0][:, :], [C.dps[0]], [dr])
            P.op(V, lambda e: e.memset(base[:, 0, :], 0.0), [], [dr])
            for t in range(1, NT):
                P.tt(V, base[:, t, :], base[:, t - 1, :], cnt[:, t - 1, :], ALU.add, [dr], [dr])
            P.tt(V, rank[:].rearrange("p t e -> p (t e)"), C.ps[1][:, :], base[:].rearrange("p t e -> p (t e)"),
                 ALU.add, [C.dps[1], dr], [d_rank])

            sidx = s_("sidx", [128, NT, NE])
            s1 = s_("s1", [128, NT]); s2 = s_("s2", [128, NT])
            slf = s_("slf", [128, NT, 2])
            P.tt(V, sidx[:], rank[:], C.ecap[:].unsqueeze(1).to_broadcast([128, NT, NE]), ALU.add,
                 [d_rank, C.d_const], [dr])
            P.ts(V, t1[:], rank[:], float(CAP), None, ALU.is_lt, None, [d_rank], [dr])
            P.tt(V, t1[:], t1[:], sel[:], ALU.mult, [dr, d_gates], [dr])
            P.tt(V, sidx[:], sidx[:], t1[:], ALU.mult, [dr], [dr])
            P.red(V, s1[:], sidx[:], ALU.max, [dr], [dr])
            P.tt(V, t1[:], sidx[:], bc(s1[:], NE), ALU.not_equal, [dr], [dr])
            P.tt(V, t1[:], t1[:], sidx[:], ALU.mult, [dr], [dr])
            P.red(V, s2[:], t1[:], ALU.max, [dr], [dr])
            P.cp(V, slf[:, :, 0], s1[:], [dr], [dr])
            P.cp(V, slf[:, :, 1], s2[:], [dr], [dr])
            P.cp(V, slot[:], slf[:], [dr], [d_slot])

            tg = s_("tg", [128, NT, NE, 2])
            P.tt(V, tg[:, :, :, 0], sel[:], C.tokid[:].unsqueeze(2).to_broadcast([128, NT, NE]), ALU.mult,
                 [d_gates, C.d_const], [dr])
            P.cp(V, tg[:, :, :, 1], gates[:], [d_gates], [dr])
            Pm = [s_("Pm%d" % i, [128, NT, CAP]) for i in range(2)]
            d_Pm = deps(2)
            iota_b = C.iota[:].unsqueeze(1).to_broadcast([128, NT, CAP])
            for e_ in range(NE):
                b = e_ % 2
                rank_b = rank[:, :, e_].unsqueeze(2).to_broadcast([128, NT, CAP])
                P.tt("vector", Pm[b][:], iota_b, rank_b, ALU.is_equal,
                     [d_rank, C.d_const], [d_Pm[b]])
                for rt in range(2):
                    for t in range(NT):
                        P.mm(C.ps[5 + rt][:, e_ * 2:e_ * 2 + 2], Pm[b][:, t, rt * 128:(rt + 1) * 128],
                             tg[:, t, e_, :], t == 0, t == NT - 1, [d_Pm[b], dr], [C.dps[5 + rt]])
            for rt in range(2):
                P.cp(V, idxg[:, :, rt, :], C.ps[5 + rt][:, 0:NE * 2].rearrange("p (e c) -> p e c", c=2),
                     [C.dps[5 + rt]], [d_idx])
            P.cp(V, idxi[:], idxg[:, :, :, 0], [d_idx], [d_idx])
            P.flush()

        d_Y = Dep()
        with ExitStack() as st2:
            zero = sbt(st2, nc, "zero", [1, D], F32)
            dz = Dep()
            P.op("gpsimd", lambda e: e.memset(zero[:], 0.0), [], [dz])
            P.ld("sync", C.moe_y[0:1, :], zero[:], [dz], [d_Y])
            wup = [sbt(st2, nc, "wup%d" % i, [128, KC, 2 * DE], BF16) for i in range(2)]
            wdn = [sbt(st2, nc, "wdn%d" % i, [128, 4, D], BF16) for i in range(2)]
            d_wup, d_wdn = deps(2), deps(2)
            xg = [sbt(st2, nc, "xg%d" % i, [128, D], F32) for i in range(3)]
            d_xg = deps(3)
            xgT = [sbt(st2, nc, "xgT%d" % i, [128, KC, CAP], BF16) for i in range(2)]
            d_xgT = deps(2)
            hT = [sbt(st2, nc, "hT%d" % i, [128, 4, CAP], BF16) for i in range(2)]
            d_hT = deps(2)
            sg = [sbt(st2, nc, "sg%d" % i, [128, CAP], F32) for i in range(2)]
            d_sg = deps(2)
            yg = [sbt(st2, nc, "yg%d" % i, [128, D], F32) for i in range(2)]
            d_yg = deps(2)
            gi = 0
            for e_ in range(NE):
                wb = e_ % 2
                P.ld("gpsimd", wup[wb][:], C.moe_w_up[C.li(L), e_].rearrange("(k p) c -> p k c", p=128), [],
                     [d_wup[wb]])
                P.ld("gpsimd", wdn[wb][:], C.moe_w_down[C.li(L), e_].rearrange("(k p) c -> p k c", p=128), [],
                     [d_wdn[wb]])
                for rt in range(2):
                    g3 = gi % 3
                    gi += 1
                    P.dma("gpsimd", lambda e, g3=g3, e_=e_, rt=rt: e.indirect_dma_start(
                        out=xg[g3][:], out_offset=None, in_=xin,
                        in_offset=bass.IndirectOffsetOnAxis(ap=idxi[:, e_, rt:rt + 1], axis=0)),
                        [d_idx] + list(d_xi), [d_xg[g3]])
                    for h in range(2):
                        ps = C.ps[2 + h]
                        dp = C.dps[2 + h]
                        for kk in range(4):
                            k = h * 4 + kk
                            P.tp(ps[:, kk * 128:(kk + 1) * 128], xg[g3][:, k * 128:(k + 1) * 128], C.ident[:],
                                 [d_xg[g3], C.d_const], [dp])
                        src = ps[:, :].rearrange("p (a b) -> p a b", a=4)
                        P.cp("vector" if h == 0 else "scalar", xgT[wb][:, h * 4:(h + 1) * 4, rt * 128:(rt + 1) * 128],
                             src, [dp], [d_xgT[wb]])
                for m in range(4):
                    i = m % 2
                    psg = C.ps[i]
                    psu = C.ps[4 + i]
                    for k in range(KC):
                        P.mm(psg[:, 0:CAP], wup[wb][:, k, m * 128:(m + 1) * 128], xgT[wb][:, k, :], k == 0,
                             k == KC - 1, [d_wup[wb], d_xgT[wb]], [C.dps[i]])
                    for k in range(KC):
                        P.mm(psu[:, 0:CAP], wup[wb][:, k, DE + m * 128:DE + (m + 1) * 128], xgT[wb][:, k, :], k == 0,
                             k == KC - 1, [d_wup[wb], d_xgT[wb]], [C.dps[4 + i]])
                    P.act(sg[i][:], psg[:, 0:CAP], AF.Silu, [C.dps[i]], [d_sg[i]])
                    P.tt(V, hT[wb][:, m, :], sg[i][:], psu[:, 0:CAP], ALU.mult, [d_sg[i], C.dps[4 + i]], [d_hT[wb]])
                for rt in range(2):
                    yb = (e_ * 2 + rt) % 2
                    for n in range(2):
                        j = 6 + n
                        psy = C.ps[j]
                        for m in range(4):
                            P.mm(psy[:, :], hT[wb][:, m, rt * 128:(rt + 1) * 128], wdn[wb][:, m, n * 512:(n + 1) * 512],
                                 m == 0, m == 3, [d_hT[wb], d_wdn[wb]], [C.dps[j]])
                        if n == 0:
                            P.ts(V, yg[yb][:, 0:512], psy[:, :], idxg[:, e_, rt, 1:2], None, ALU.mult, None,
                                 [C.dps[j], d_idx], [d_yg[yb]])
                        else:
                            P.act(yg[yb][:, 512:1024], psy[:, :], AF.Copy, [C.dps[j], d_idx], [d_yg[yb]],
                                  scale=idxg[:, e_, rt, 1:2])
                    r0 = 1 + e_ * CAP + rt * 128
                    P.ld("sync", C.moe_y[r0:r0 + 128, :], yg[yb][:], [d_yg[yb]], [d_Y])
            P.flush()

        with ExitStack() as st3:
            gam, bet, d_gb = emit_load_gb(P, C, st3, L, 1)
            stats = [sbt(st3, nc, "mst%d" % i, [128, 2, 6], F32) for i in range(2)]
            mv = [sbt(st3, nc, "mmv%d" % i, [128, 4], F32) for i in range(2)]
            d_st, d_mv = deps(2), deps(2)
            yk = [[sbt(st3, nc, "yk%d%d" % (i, k), [128, D], F32) for k in range(2)] for i in range(2)]
            d_yk = [deps(2), deps(2)]
            for t in range(NT):
                b = t % 2
                for k in range(2):
                    P.dma("gpsimd", lambda e, b=b, k=k, t=t: e.indirect_dma_start(
                        out=yk[b][k][:], out_offset=None, in_=C.moe_y,
                        in_offset=bass.IndirectOffsetOnAxis(ap=slot[:, t, k:k + 1], axis=0)),
                        [d_slot, d_Y], [d_yk[b][k]])
                    P.tt(V if k == 0 else "gpsimd", acc[:, t, :], acc[:, t, :], yk[b][k][:], ALU.add,
                         [d_acc[t], d_yk[b][k]], [d_acc[t]])
                emit_layernorm_store(P, C, (stats[b], d_st[b], mv[b], d_mv[b]), acc[:, t, :], d_acc[t], gam[:], bet[:],
                                     d_gb, xout, d_xo[t], t)
            P.flush()


def emit_conv(P, C, L, xin, d_xi, xout, d_xo):
    nc = P.nc
    j = L // 2
    with ExitStack() as st:
        xTe = sbt(st, nc, "xTe", [128, KC, TOK + 2], BF16)
        d_xT = Dep()
        zT = sbt(st, nc, "zT", [128, KC, TOK], BF16)
        d_z = Dep()
        d_hx, d_hxa, d_halo = Dep(), Dep(), Dep()
        P.ld("sync", C.hx_in[0:1, :], xin[0:1, :], [d_xi[0]], [d_hx])
        P.ld("sync", C.hx_in[1:2, :], xin[TOK - 1:TOK, :], [d_xi[NT - 1]], [d_hx])
        P.coll(lambda e: e.collective_compute("AllGather", ALU.bypass, replica_groups=[list(range(NCORES))],
                                              ins=[C.hx_in.opt()], outs=[C.hx_all.opt()]), [d_hx], [d_hxa])
        halo = sbt(st, nc, "halo", [16, D], F32)
        P.ld("sync", halo[:], C.hx_all, [d_hxa], [d_halo])
        for k in range(KC):
            P.mm(C.ps[4][:, 2 * k:2 * k + 2], halo[0:16, k * 128:(k + 1) * 128], C.selc[0:16, :], True, True,
                 [d_halo, C.d_const], [C.dps[4]])
        P.cp("vector", xTe[:, :, 0:TOK + 2:TOK + 1], C.ps[4][:, 0:16].rearrange("p (k two) -> p k two", two=2),
             [C.dps[4]], [d_xT])
        cwr = sbt(st, nc, "cwr", [24, 128], F32)
        cw = sbt(st, nc, "cw", [128, 24], F32)
        d_cw = Dep()
        P.ld("sync", cwr[:], C.cv_w[C.ji(j)].rearrange("k (m p) -> (k m) p", p=128), [], [d_cw])
        P.tp(C.ps[5][:, 0:24], cwr[0:24, :], C.ident[0:24, 0:24], [d_cw, C.d_const], [C.dps[5]])
        P.cp("vector", cw[:], C.ps[5][:, 0:24], [C.dps[5]], [d_cw])

        emit_load_xT(P, C, st, xin, d_xi, xTe, d_xT, 1)

        wc = [sbt(st, nc, "wc%d" % i, [128, KC, 3, 128], BF16) for i in range(2)]
        d_wc = deps(2)
        u = sbt(st, nc, "u", [128, TOK + 2], F32)
        d_u = Dep()
        bgs = sbt(st, nc, "bgs", [128, TOK], F32)
        d_bg = Dep()
        y = sbt(st, nc, "y", [128, TOK], F32)
        d_yy = Dep()
        tmpc = [sbt(st, nc, "tmpc%d" % i, [128, 512], F32) for i in range(2)]
        d_tc = deps(2)
        tmp4 = sbt(st, nc, "tmp4", [128, 4], F32)
        d_t4 = Dep()
        w_in = C.cv_w_in[C.ji(j)].rearrange("(k p) (s c) -> p k s c", p=128, s=3)
        banks = [(0, 1, 2), (3, 6, 7)]
        for m in range(KC):
            wb = m % 2
            for s_ in range(3):
                P.ld("gpsimd", wc[wb][:, :, s_, :], w_in[:, :, s_, m * 128:(m + 1) * 128], [], [d_wc[wb]])
            for s_ in (1, 2):
                for k in range(KC):
                    P.mm(C.ps[5][:, (s_ - 1) * 2:(s_ - 1) * 2 + 2], wc[wb][:, k, s_, :], xTe[:, k, 0:TOK + 2:TOK + 1],
                         k == 0, k == KC - 1, [d_wc[wb], d_xT], [C.dps[5]])
            P.cp("scalar", tmp4[:], C.ps[5][:, 0:4], [C.dps[5]], [d_t4])
            P.tt("vector", u[:, 0:TOK + 2:TOK + 1], tmp4[:, 0:2], tmp4[:, 2:4], ALU.mult, [d_t4], [d_u])
            for g in range(4):
                bk = banks[g % 2]
                lo = 1 + g * 512
                for si, s_ in enumerate((0, 1, 2)):
                    for k in range(KC):
                        P.mm(C.ps[bk[si]][:, :], wc[wb][:, k, s_, :], xTe[:, k, lo:lo + 512], k == 0, k == KC - 1,
                             [d_wc[wb], d_xT], [C.dps[bk[si]]])
                i = g % 2
                P.cp("scalar", tmpc[i][:], C.ps[bk[1]][:, :], [C.dps[bk[1]]], [d_tc[i]])
                P.tt("vector", u[:, lo:lo + 512], tmpc[i][:], C.ps[bk[2]][:, :], ALU.mult, [d_tc[i], C.dps[bk[2]]],
                     [d_u])
                P.cp("scalar", bgs[:, g * 512:(g + 1) * 512], C.ps[bk[0]][:, :], [C.dps[bk[0]]], [d_bg])
            P.ts("gpsimd", y[:], u[:, 0:TOK], cw[:, m:m + 1], None, ALU.mult, None, [d_u, d_cw], [d_yy])
            P.stt("vector", y[:], u[:, 1:TOK + 1], cw[:, 8 + m:9 + m], y[:], ALU.mult, ALU.add, [d_u, d_cw, d_yy],
                  [d_yy])
            P.stt("vector", y[:], u[:, 2:TOK + 2], cw[:, 16 + m:17 + m], y[:], ALU.mult, ALU.add, [d_u, d_cw, d_yy],
                  [d_yy])
            P.tt("vector", zT[:, m, :], y[:], bgs[:], ALU.mult, [d_yy, d_bg], [d_z])
        emit_mixer_tail(P, C, st, L, zT, d_z, C.cv_w_out[C.ji(j)], xin, d_xi, xout, d_xo)
        P.flush()


def emit_hgrn(P, C, L, xin, d_xi, xout, d_xo):
    nc = P.nc
    j = L // 2
    NH = 8
    G3 = [128, NCH, CH]
    with ExitStack() as st_outer:
        xT = sbt(st_outer, nc, "hxT", [128, KC, TOK], BF16)
        d_xT = Dep()
        lbc = sbt(st_outer, nc, "lbc", [128, 16], F32)
        oml = sbt(st_outer, nc, "oml", [128, 16], F32)
        lbm1 = sbt(st_outer, nc, "lbm1", [128, 16], F32)
        nw = sbt(st_outer, nc, "nw", [128, 1], F32)
        d_lb = Dep()
        with ExitStack() as st:
            lgt = sbt(st, nc, "lgt", [64, 128], F32)
            lbT = sbt(st, nc, "lbT", [128, 4, 16], F32)
            mx = sbt(st, nc, "lmx", [128, 16], F32)
            mx2 = sbt(st, nc, "lmx2", [128, 16], F32)
            ssum = sbt(st, nc, "lss", [128, 16], F32)
            P.ld("sync", lgt[:], C.hg_lb_logits.rearrange("l d (h p) -> (l d h) p", p=128), [], [d_lb])
            P.ld("sync", nw[:], C.hg_norm_w[C.ji(j)].rearrange("(p o) -> p o", o=1), [], [d_lb])
            P.tp(C.ps[4][:, 0:64], lgt[0:64, :], C.ident[0:64, 0:64], [d_lb, C.d_const], [C.dps[4]])
            P.cp("vector", lbT[:].rearrange("p l r -> p (l r)"), C.ps[4][:, 0:64], [C.dps[4]], [d_lb])
            V = "vector"
            P.tt(V, mx[:], lbT[:, 0, :], lbT[:, 1, :], ALU.max, [d_lb], [d_lb])
            P.tt(V, mx2[:], lbT[:, 2, :], lbT[:, 3, :], ALU.max, [d_lb], [d_lb])
            P.tt(V, mx[:], mx[:], mx2[:], ALU.max, [d_lb], [d_lb])
            P.tt(V, lbT[:], lbT[:], mx[:].unsqueeze(1).to_broadcast([128, 4, 16]), ALU.subtract, [d_lb], [d_lb])
            P.act(lbT[:], lbT[:], AF.Exp, [d_lb], [d_lb])
            P.tt(V, ssum[:], lbT[:, 0, :], lbT[:, 1, :], ALU.add, [d_lb], [d_lb])
            P.tt(V, ssum[:], ssum[:], lbT[:, 2, :], ALU.add, [d_lb], [d_lb])
            P.tt(V, ssum[:], ssum[:], lbT[:, 3, :], ALU.add, [d_lb], [d_lb])
            P.op(V, lambda e: e.reciprocal(ssum[:], ssum[:]), [d_lb], [d_lb])
            P.op(V, lambda e: e.memset(lbc[:], 0.0), [], [d_lb])
            for i in range(1, L + 1):
                P.tt(V, lbc[:], lbc[:], lbT[:, i, :], ALU.add, [d_lb], [d_lb])
            P.tt(V, lbc[:], lbc[:], ssum[:], ALU.mult, [d_lb], [d_lb])
            P.ts(V, oml[:], lbc[:], -1.0, 1.0, ALU.mult, ALU.add, [d_lb], [d_lb])
            P.ts(V, lbm1[:], lbc[:], -1.0, None, ALU.add, None, [d_lb], [d_lb])

            emit_load_xT(P, C, st, xin, d_xi, xT, d_xT, 0)

            wh = sbt(st, nc, "wh", [128, KC, 5, 128], BF16)
            d_wh = Dep()
            qT = sbt(st, nc, "qT", [128, TOK], BF16)
            kT = [sbt(st, nc, "kT%d" % d, [128, TOK], BF16) for d in range(2)]
            Gb = [sbt(st, nc, "G%d" % d, [128, TOK + 1], F32) for d in range(2)]
            vT = sbt(st, nc, "vT", [128, TOK], BF16)
            sgT = sbt(st, nc, "sgT", [128, TOK], BF16)
            d_q, d_v, d_sg = Dep(), Dep(), Dep()
            d_k, d_G = deps(2), deps(2)
            X = sbt(st, nc, "X", [128, TOK], F32)
            d_X = Dep()
            E = [sbt(st, nc, "E%d" % i, [128, TOK], F32) for i in range(2)]
            d_E = deps(2)
            ones = sbt(st, nc, "ones", [128, TOK], BF16)
            d_ones = Dep()
            qt = [sbt(st, nc, "qt%d" % d, [128, TOK], BF16) for d in range(2)]
            kt = [sbt(st, nc, "kt%d" % d, [128, TOK], BF16) for d in range(2)]
            qh = [sbt(st, nc, "qh%d" % d, [128, TOK], BF16) for d in range(2)]
            khT = [sbt(st, nc, "khT%d" % d, [128, TOK], BF16) for d in range(2)]
            qhp = [sbt(st, nc, "qhp%d" % d, [128, TOK], BF16) for d in range(2)]
            d_qt, d_kt, d_qh, d_khT, d_qhp = deps(2), deps(2), deps(2), deps(2), deps(2)
            dcol = [sbt(st, nc, "dcol%d" % d, [128, NCH], F32) for d in range(2)]
            d_dc = deps(2)
            v_tm = sbt(st, nc, "v_tm", [CH, NCH, 128], BF16)
            kh_tm = [sbt(st, nc, "kh_tm%d" % d, [CH, NCH, 128], BF16) for d in range(2)]
            d_vtm = Dep()
            d_khtm = deps(2)
            osum = sbt(st, nc, "osum", [CH, NCH, 128], F32)
            d_os = deps(NCH)
            S = [sbt(st, nc, "S%d" % d, [128, 129], F32) for d in range(2)]
            Sb = [sbt(st, nc, "Sb%d" % d, [128, 128], BF16) for d in range(2)]
            d_S, d_Sb = deps(2), deps(2)
            A_sb = [[sbt(st, nc, "A%d%d" % (d, i), [CH, CH], BF16) for i in range(2)] for d in range(2)]
            d_A = [deps(2), deps(2)]
            d_pA = [deps(2), deps(2)]
            d_pO = [deps(2), deps(2)]
            d_pS = [deps(2), deps(2)]
            masks = [C.maskf, C.maskb]
            psb = [C.ps[i][:].bitcast(BF16) for i in range(8)]
            P.op("gpsimd", lambda e: e.memset(ones[:], 1.0), [], [d_ones])
            for d in range(2):
                P.op("gpsimd", lambda e, d=d: e.memset(Gb[d][:, 0:1], 0.0), [], [d_G[d]])
            w_in = C.hg_w_in[C.ji(j)].rearrange("(k p) (s c) -> p k s c", p=128, s=5)

            for h in range(NH):
                for s_ in range(5):
                    P.ld("gpsimd", wh[:, :, s_, :], w_in[:, :, s_, h * 128:(h + 1) * 128], [], [d_wh])
                pi = 0
                for s_ in (0, 4, 3, 1, 2):
                    for g in range(4):
                        bk = pi % 4
                        pi += 1
                        ps = C.ps[bk]
                        for k in range(KC):
                            P.mm(ps[:, :], wh[:, k, s_, :], xT[:, k, g * 512:(g + 1) * 512], k == 0, k == KC - 1,
                                 [d_wh, d_xT], [C.dps[bk]])
                        sl = slice(g * 512, (g + 1) * 512)
                        if s_ == 0:
                            P.act(qT[:, sl], ps[:, :], AF.Silu, [C.dps[bk]], [d_q])
                        elif s_ == 4:
                            P.act(sgT[:, sl], ps[:, :], AF.Silu, [C.dps[bk]], [d_sg])
                        elif s_ == 3:
                            P.cp("vector", vT[:, sl], ps[:, :], [C.dps[bk]], [d_v])
                        else:
                            P.act(X[:, sl], ps[:, :], AF.Sigmoid, [C.dps[bk]], [d_X])
                    if s_ in (1, 2):
                        d = s_ - 1
                        col = d * 8 + h
                        P.ts("vector", kT[d][:], X[:], lbm1[:, col:col + 1], oml[:, col:col + 1], ALU.mult, ALU.add,
                             [d_X, d_lb], [d_k[d]])
                        P.act(Gb[d][:, 1:TOK + 1], X[:], AF.Ln, [d_X, d_lb], [d_G[d]], bias=lbc[:, col:col + 1],
                              scale=oml[:, col:col + 1])
                        P.op("vector", lambda e, d=d: e.tensor_tensor_scan(Gb[d][:, 1:TOK + 1], ones[:],
                                                                            Gb[d][:, 1:TOK + 1], 0.0, ALU.mult, ALU.add),
                             [d_ones, d_G[d]], [d_G[d]])
                ei = 0
                for d in range(2):
                    sgn = 1.0 if d == 0 else -1.0
                    Vw = Gb[d][:, 1:TOK + 1] if d == 0 else Gb[d][:, 0:TOK]
                    V3 = Vw.rearrange("p (c i) -> p c i", i=CH)
                    X3 = X[:].rearrange("p (c i) -> p c i", i=CH)
                    refA = Gb[d][:, 0:TOK:CH]
                    refM = Gb[d][:, CH // 2:TOK:CH]
                    refB = Gb[d][:, CH:TOK + 1:CH]

                    def bc(r):
                        return r.unsqueeze(2).to_broadcast(G3)

                    def fac(dst, d_dst, src, d_src, scale, from_x=True, bias=None, Vin=None):
                        nonlocal ei
                        b = ei % 2
                        ei += 1
                        if from_x:
                            P.act(E[b][:], X[:], AF.Exp, [d_X], [d_E[b]], scale=scale)
                        else:
                            P.act(E[b][:], Vin, AF.Exp, [d_G[d]], [d_E[b]], scale=scale, bias=bias)
                        P.tt("gpsimd" if ei % 2 == 0 else "vector", dst[:], src[:], E[b][:], ALU.mult, [d_src, d_E[b]],
                             [d_dst])

                    P.tt("vector", X3, V3, bc(refM), ALU.subtract, [d_G[d]], [d_X])
                    fac(qt[d], d_qt[d], qT, d_q, sgn)
                    fac(kt[d], d_kt[d], kT[d], d_k[d], -sgn)
                    rq, rk = (refA, refB) if d == 0 else (refB, refA)
                    P.tt("vector", X3, V3, bc(rq), ALU.subtract, [d_G[d]], [d_X])
                    fac(qh[d], d_qh[d], qT, d_q, sgn)
                    P.tt("vector", X3, V3, bc(rk), ALU.subtract, [d_G[d]], [d_X])
                    fac(khT[d], d_khT[d], kT[d], d_k[d], -sgn)
                    if d == 0:
                        fac(qhp[d], d_qhp[d], qT, d_q, 1.0, from_x=False, bias=0.0, Vin=Vw)
                    else:
                        fac(qhp[d], d_qhp[d], qT, d_q, -1.0, from_x=False, bias=Gb[d][:, TOK:TOK + 1], Vin=Vw)
                    P.tt("vector", dcol[d][:], refB, refA, ALU.subtract, [d_G[d]], [d_dc[d]])
                    P.act(dcol[d][:], dcol[d][:], AF.Exp, [d_dc[d]], [d_dc[d]])
                    P.act(S[d][:, 128:129], Gb[d][:, TOK:TOK + 1], AF.Exp, [d_G[d]], [d_S[d]])
                    P.op("gpsimd", lambda e, d=d: e.memset(S[d][:, 0:128], 0.0), [], [d_S[d]])
                    P.op("gpsimd", lambda e, d=d: e.memset(Sb[d][:], 0.0), [], [d_Sb[d]])
                ti = 0
                for (srcT, d_src, dst, d_dst) in ((vT, d_v, v_tm, d_vtm), (khT[0], d_khT[0], kh_tm[0], d_khtm[0]),
                                                 (khT[1], d_khT[1], kh_tm[1], d_khtm[1])):
                    for c4 in range(NCH // 4):
                        bk = ti % 4
                        ti += 1
                        for i in range(4):
                            c = c4 * 4 + i
                            P.tp(psb[bk][0:CH, i * 128:(i + 1) * 128], srcT[:, c * CH:(c + 1) * CH], C.identb[:],
                                 [d_src, C.d_const], [C.dps[bk]])
                        src = psb[bk][0:CH, 0:512].rearrange("p (a b) -> p a b", a=4)
                        eng = "scalar" if ti % 2 == 0 else "vector"
                        P.cp(eng, dst[:, c4 * 4:(c4 + 1) * 4, :], src, [C.dps[bk]], [d_dst])
                written = set()
                for s_ in range(NCH):
                    par = s_ % 2
                    for d in range(2):
                        c = s_ if d == 0 else NCH - 1 - s_
                        cs = slice(c * CH, (c + 1) * CH)
                        r = d * 2 + par
                        pA = C.ps[4][0:CH, r * CH:(r + 1) * CH]
                        pO = C.ps[5][0:CH, r * 128:(r + 1) * 128]
                        pS = C.ps[6][:, r * 128:(r + 1) * 128]
                        P.mm(pA, kt[d][:, cs], qt[d][:, cs], True, True, [d_kt[d], d_qt[d]], [d_pA[d][par]])
                        P.tt("vector", A_sb[d][par][:], pA, masks[d][:], ALU.mult, [d_pA[d][par], C.d_const],
                             [d_A[d][par]])
                        P.mm(pO, A_sb[d][par][:], v_tm[:, c, :], True, False, [d_A[d][par], d_vtm], [d_pO[d][par]])
                        P.mm(pO, qh[d][:, cs], Sb[d][:], False, True, [d_qh[d], d_Sb[d], d_A[d][par], d_vtm],
                             [d_pO[d][par]])
                        if c not in written:
                            written.add(c)
                            P.cp("scalar", osum[:, c, :], pO, [d_pO[d][par]], [d_os[c]])
                        else:
                            P.tt("vector", osum[:, c, :], osum[:, c, :], pO, ALU.add, [d_pO[d][par], d_os[c]],
                                 [d_os[c]])
                        P.mm(pS, kh_tm[d][:, c, :], v_tm[:, c, :], True, True, [d_khtm[d], d_vtm], [d_pS[d][par]])
                        P.stt("vector", S[d][:, 0:128], S[d][:, 0:128], dcol[d][:, c:c + 1], pS, ALU.mult, ALU.add,
                              [d_S[d], d_dc[d], d_pS[d][par]], [d_S[d]])
                        if s_ < NCH - 1:
                            P.cp("scalar", Sb[d][:], S[d][:, 0:128], [d_S[d]], [d_Sb[d]])
                P.ld("sync", C.hg_oloc[h], osum[:].rearrange("p c v -> p (c v)"), d_os, [C.d_oloc[h]])
                P.ld("sync", C.hg_sg[h], sgT[:], [d_sg], [C.d_sgd[h]])
                for d in range(2):
                    P.ld("sync", C.hg_qhp[h * 2 + d], qhp[d][:], [d_qhp[d]], [C.d_qhpd[h * 2 + d]])
                    col = (d * 8 + h) * 129
                    P.ld("sync", C.hg_xin[:, col:col + 129], S[d][:], [d_S[d]], [C.d_xch])
            P.flush()
        d_xall = Dep()
        P.coll(lambda e: e.collective_compute("AllGather", ALU.bypass, replica_groups=[list(range(NCORES))],
                                              ins=[C.hg_xin.opt()], outs=[C.hg_xall.opt()]), [], [d_xall])
        with ExitStack() as st:
            oT = xT
            d_oT = Dep()
            SG = [sbt(st, nc, "SG%d" % i, [128, NCORES, 2, 129], F32) for i in range(2)]
            d_SG = deps(2)
            Sin = [sbt(st, nc, "Sin%d" % d, [128, 128], F32) for d in range(2)]
            Stmp = [sbt(st, nc, "Stmp%d" % d, [128, 128], F32) for d in range(2)]
            Sinb = [sbt(st, nc, "Sinb%d" % d, [128, 128], BF16) for d in range(2)]
            d_Sin, d_Stmp, d_Sinb = deps(2), deps(2), deps(2)
            ol = [sbt(st, nc, "ol%d" % i, [CH, NCH, 128], F32) for i in range(2)]
            d_ol = deps(2)
            sq = sbt(st, nc, "sq", [CH, NCH, 128], F32)
            d_sq = Dep()
            ss = sbt(st, nc, "ss", [CH, NCH], F32)
            d_ss = Dep()
            onb = sbt(st, nc, "onb", [CH, NCH, 128], BF16)
            d_on = Dep()
            qp = [[sbt(st, nc, "qp%d%d" % (i, d), [128, TOK], BF16) for d in range(2)] for i in range(2)]
            d_qp = [deps(2), deps(2)]
            sgl = [sbt(st, nc, "sgl%d" % i, [128, TOK], BF16) for i in range(2)]
            d_sgl = deps(2)
            psb = [C.ps[i][:].bitcast(BF16) for i in range(8)]
            xall = C.hg_xall.rearrange("(jj p) f -> p jj f", p=128)
            for h in range(NH):
                b = h % 2
                for d in range(2):
                    col = (d * 8 + h) * 129
                    P.ld("sync", SG[b][:, :, d, :], xall[:, :, col:col + 129], [d_xall], [d_SG[b]])
                    P.ld("sync", qp[b][d][:], C.hg_qhp[h * 2 + d], [C.d_qhpd[h * 2 + d]], [d_qp[b][d]])
                P.ld("sync", ol[b][:].rearrange("p c v -> p (c v)"), C.hg_oloc[h], [C.d_oloc[h]], [d_ol[b]])
                P.ld("sync", sgl[b][:], C.hg_sg[h], [C.d_sgd[h]], [d_sgl[b]])
                for d in range(2):
                    eng = "vector"
                    P.op(eng, lambda e, d=d: e.memset(Sin[d][:], 0.0), [], [d_Sin[d]])
                    order = range(NCORES) if d == 0 else range(NCORES - 1, -1, -1)
                    for jj in order:
                        m = C.coef[:, d * 16 + jj:d * 16 + jj + 1]
                        om = C.coef[:, d * 16 + 8 + jj:d * 16 + 9 + jj]
                        P.stt(eng, Stmp[d][:], Sin[d][:], SG[b][:, jj, d, 128:129], SG[b][:, jj, d, 0:128], ALU.mult,
                              ALU.add, [d_Sin[d], d_SG[b]], [d_Stmp[d]])
                        P.ts(eng, Sin[d][:], Sin[d][:], om, None, ALU.mult, None, [d_Sin[d], C.d_const], [d_Sin[d]])
                        P.stt(eng, Sin[d][:], Stmp[d][:], m, Sin[d][:], ALU.mult, ALU.add,
                              [d_Stmp[d], d_Sin[d], C.d_const], [d_Sin[d]])
                    P.cp("scalar", Sinb[d][:], Sin[d][:], [d_Sin[d]], [d_Sinb[d]])
                for c4 in range(NCH // 4):
                    bk = c4 % 2
                    for i in range(4):
                        c = c4 * 4 + i
                        cs = slice(c * CH, (c + 1) * CH)
                        pc = C.ps[bk][0:CH, i * 128:(i + 1) * 128]
                        P.mm(pc, qp[b][0][:, cs], Sinb[0][:], True, False, [d_qp[b][0], d_Sinb[0]], [C.dps[bk]])
                        P.mm(pc, qp[b][1][:, cs], Sinb[1][:], False, True,
                             [d_qp[b][1], d_Sinb[1], d_qp[b][0], d_Sinb[0]], [C.dps[bk]])
                    o4 = ol[b][:, c4 * 4:(c4 + 1) * 4, :]
                    P.tt("vector", o4, o4, C.ps[bk][0:CH, :].rearrange("p (a v) -> p a v", a=4), ALU.add,
                         [d_ol[b], C.dps[bk]], [d_ol[b]])
                P.tt("gpsimd", sq[:], ol[b][:], ol[b][:], ALU.mult, [d_ol[b]], [d_sq])
                P.red("vector", ss[:], sq[:], ALU.add, [d_sq], [d_ss])
                P.ts("vector", ss[:], ss[:], 1.0 / 128.0, RMS_EPS, ALU.mult, ALU.add, [d_ss], [d_ss])
                P.act(ss[:], ss[:], AF.Sqrt, [d_ss], [d_ss])
                P.op("vector", lambda e: e.reciprocal(ss[:], ss[:]), [d_ss], [d_ss])
                P.tt("vector", onb[:], ol[b][:], ss[:].unsqueeze(2).to_broadcast([CH, NCH, 128]), ALU.mult,
                     [d_ol[b], d_ss], [d_on])
                for c8 in range(NCH // 8):
                    bk = 2 + c8 % 2
                    for i in range(8):
                        c = c8 * 8 + i
                        P.tp(psb[bk][:, i * CH:(i + 1) * CH], onb[0:CH, c, :], C.identb[0:CH, 0:CH], [d_on, C.d_const],
                             [C.dps[bk]])
                    tk = slice(c8 * 512, (c8 + 1) * 512)
                    P.stt("vector", oT[:, h, tk], psb[bk][:, 0:512], nw[:, 0:1], sgl[b][:, tk], ALU.mult, ALU.mult,
                          [C.dps[bk], d_lb, d_sgl[b]], [d_oT])
            emit_mixer_tail(P, C, st, L, oT, d_oT, C.hg_w_out[C.ji(j)], xin, d_xi, xout, d_xo)
            P.flush()


def w_specs(slim):
    nl = DEPTH if slim is None else 1
    nj = 2 if slim is None else 1
    return [
        ("hg_w_in", [nj, D, 5 * D]), ("hg_lb_logits", [DEPTH, 2, D]), ("hg_norm_w", [nj, 128]),
        ("hg_w_out", [nj, D, D]), ("cv_w_in", [nj, D, 3 * D]), ("cv_w", [nj, 3, D]), ("cv_w_out", [nj, D, D]),
        ("ln_g", [DEPTH, 2, D]), ("ln_b", [DEPTH, 2, D]), ("moe_w_group", [nl, D, 4]), ("moe_b_group", [nl, 4]),
        ("moe_w_expert", [nl, D, NE]), ("moe_b_expert", [nl, NE]), ("moe_w_up", [nl, NE, D, 2 * DE]),
        ("moe_w_down", [nl, NE, DE, D]),
    ]


LAYER_KEYS = ("moe_w_group", "moe_b_group", "moe_w_expert", "moe_b_expert", "moe_w_up", "moe_w_down")
MIXER_KEYS = ("hg_w_in", "hg_norm_w", "hg_w_out", "cv_w_in", "cv_w", "cv_w_out")
C_SPECS = [("c_ident", [128, 128]), ("c_maskf", [CH, CH]), ("c_maskb", [CH, CH]), ("c_coef", [128, 32]),
           ("c_selc", [16, 2]), ("c_ustrict", [128, 128]), ("c_iota", [128, CAP]), ("c_tokid", [128, NT]),
           ("c_ecap", [128, NE])]

FULL_STAGES = []
for _l in range(DEPTH):
    FULL_STAGES.append(("hgrn" if _l % 2 == 0 else "conv", _l))
    FULL_STAGES.append(("moe", _l))


def build(stages, slim=None):
    nc = bass.Bass("TRN2", target_bir_lowering=False)
    C = Ctx()
    C.li = (lambda l: l) if slim is None else (lambda l: 0)
    C.ji = (lambda j: j) if slim is None else (lambda j: 0)
    C.x = nc.dram_tensor("x", [TOK, D], F32, kind="ExternalInput").ap()
    for name, shape in w_specs(slim) + C_SPECS:
        setattr(C, name, nc.dram_tensor(name, shape, F32, kind="ExternalInput").ap())
    C.out = nc.dram_tensor("out", [TOK, D], F32, kind="ExternalOutput").ap()
    xs = [nc.dram_tensor("xs%d" % i, [TOK, D], F32, kind="Internal").ap() for i in range(2)]
    C.hg_oloc = [nc.dram_tensor("hg_oloc%d" % h, [CH, NCH * 128], F32, kind="Internal").ap() for h in range(8)]
    C.hg_sg = [nc.dram_tensor("hg_sg%d" % h, [128, TOK], BF16, kind="Internal").ap() for h in range(8)]
    C.hg_qhp = [nc.dram_tensor("hg_qhp%d" % h, [128, TOK], BF16, kind="Internal").ap() for h in range(16)]
    C.hg_xin = nc.dram_tensor("hg_xin", [128, 16 * 129], F32, kind="Internal").ap()
    C.hg_xall = nc.dram_tensor("hg_xall", [128 * NCORES, 16 * 129], F32, kind="Internal").ap()
    C.d_oloc, C.d_sgd, C.d_qhpd, C.d_xch = deps(8), deps(8), deps(16), Dep()
    C.moe_y = nc.dram_tensor("moe_y", [1 + NE * CAP, D], F32, kind="Internal").ap()
    C.hx_in = nc.dram_tensor("hx_in", [2, D], F32, kind="Internal").ap()
    C.hx_all = nc.dram_tensor("hx_all", [2 * NCORES, D], F32, kind="Internal").ap()
    with ExitStack() as gst:
        C.ps = [gst.enter_context(nc.psum_tensor("ps%d" % i, [128, 512], F32)) for i in range(8)]
        C.dps = deps(8)
        C.ident = sbt(gst, nc, "ident", [128, 128], F32)
        C.identb = sbt(gst, nc, "identb", [128, 128], BF16)
        C.maskf = sbt(gst, nc, "maskf", [CH, CH], F32)
        C.maskb = sbt(gst, nc, "maskb", [CH, CH], F32)
        C.coef = sbt(gst, nc, "coef", [128, 32], F32)
        C.selc = sbt(gst, nc, "selc", [16, 2], F32)
        C.ustrict = sbt(gst, nc, "ustrict", [128, 128], F32)
        C.iota = sbt(gst, nc, "iota", [128, CAP], F32)
        C.tokid = sbt(gst, nc, "tokid", [128, NT], F32)
        C.ecap = sbt(gst, nc, "ecap", [128, NE], F32)
        C.d_const = Dep()
        P = Prog(nc)
        P.ld("sync", C.ident[:], C.c_ident, [], [C.d_const])
        P.ld("sync", C.maskf[:], C.c_maskf, [], [C.d_const])
        P.ld("sync", C.maskb[:], C.c_maskb, [], [C.d_const])
        P.ld("sync", C.coef[:], C.c_coef, [], [C.d_const])
        P.ld("sync", C.selc[:], C.c_selc, [], [C.d_const])
        P.ld("sync", C.ustrict[:], C.c_ustrict, [], [C.d_const])
        P.ld("sync", C.iota[:], C.c_iota, [], [C.d_const])
        P.ld("sync", C.tokid[:], C.c_tokid, [], [C.d_const])
        P.ld("sync", C.ecap[:], C.c_ecap, [], [C.d_const])
        P.cp("vector", C.identb[:], C.ident[:], [C.d_const], [C.d_const])
        P.flush()
        cur, d_cur = C.x, deps(NT)
        for i, (kind, L) in enumerate(stages):
            last = i == len(stages) - 1
            dst = C.out if last else xs[i % 2]
            d_dst = deps(NT)
            EMIT[kind](P, C, L, cur, d_cur, dst, d_dst)
            cur, d_cur = dst, d_dst
        P.finish()
    return nc


def host_consts(c):
    b, p = divmod(c, 4)
    ident = np.eye(128, dtype=np.float32)
    jj128, ii128 = np.meshgrid(np.arange(128), np.arange(128), indexing="ij")
    jj, ii = np.meshgrid(np.arange(CH), np.arange(CH), indexing="ij")
    maskf = (jj <= ii).astype(np.float32)
    maskb = (jj >= ii).astype(np.float32)
    coef = np.zeros((128, 32), np.float32)
    for j in range(8):
        same = (j // 4) == b
        mf = 1.0 if (same and j < c) else 0.0
        mb = 1.0 if (same and j > c) else 0.0
        coef[:, j] = mf
        coef[:, 8 + j] = 1.0 - mf
        coef[:, 16 + j] = mb
        coef[:, 24 + j] = 1.0 - mb
    selc = np.zeros((16, 2), np.float32)
    if p > 0:
        selc[2 * (c - 1) + 1, 0] = 1.0
    if p < 3:
        selc[2 * (c + 1), 1] = 1.0
    ustrict = (jj128 < ii128).astype(np.float32)
    iota = np.tile(np.arange(CAP, dtype=np.float32)[None, :], (128, 1))
    tokid = (np.arange(NT, dtype=np.float32)[None, :] * 128 + np.arange(128, dtype=np.float32)[:, None])
    ecap = np.tile((np.arange(NE, dtype=np.float32) * CAP + 1.0)[None, :], (128, 1))
    return {"c_ident": ident, "c_maskf": maskf, "c_maskb": maskb, "c_coef": coef, "c_selc": selc,
            "c_ustrict": ustrict, "c_iota": iota, "c_tokid": np.ascontiguousarray(tokid), "c_ecap": ecap}


def run_stages(stages, x_full, weights, slim=None):
    nc = build(stages, slim)
    xf = np.ascontiguousarray(x_full, dtype=np.float32).reshape(NCORES, TOK, D)
    wl = {}
    for name, shape in w_specs(slim):
        a = weights[name]
        if slim is not None and name in LAYER_KEYS:
            a = a[slim:slim + 1]
        if slim is not None and name in MIXER_KEYS:
            a = a[slim // 2:slim // 2 + 1]
        wl[name] = np.ascontiguousarray(a, dtype=np.float32).reshape(shape)
    in_maps = []
    for c in range(NCORES):
        m = {"x": xf[c]}
        m.update(wl)
        m.update(host_consts(c))
        in_maps.append(m)
    res = run_bass_kernel_spmd(nc, in_maps, core_ids=list(range(NCORES)))
    out = np.stack([np.asarray(res.results[c]["out"]) for c in range(NCORES)], axis=0)
    return out.reshape(2, 4 * TOK, D).astype(np.float32)


def kernel(x, hg_w_in, hg_lb_logits, hg_norm_w, hg_w_out, cv_w_in, cv_w, cv_w_out, ln_g, ln_b,
           moe_w_group, moe_b_group, moe_w_expert, moe_b_expert, moe_w_up, moe_w_down):
    weights = dict(hg_w_in=hg_w_in, hg_lb_logits=hg_lb_logits, hg_norm_w=hg_norm_w, hg_w_out=hg_w_out,
                   cv_w_in=cv_w_in, cv_w=cv_w, cv_w_out=cv_w_out, ln_g=ln_g, ln_b=ln_b, moe_w_group=moe_w_group,
                   moe_b_group=moe_b_group, moe_w_expert=moe_w_expert, moe_b_expert=moe_b_expert,
                   moe_w_up=moe_w_up, moe_w_down=moe_w_down)
    return run_stages(FULL_STAGES, np.asarray(x), {k: np.asarray(v) for k, v in weights.items()})


EMIT = {"moe": emit_moe_sparse, "moed": emit_moe, "conv": emit_conv, "hgrn": emit_hgrn}
```
